# Optimizing a Trainium2 kernel written in Bass

```python
import math
import jax, jax.numpy as jnp
from jax import lax
import numpy as np

D_MODEL = 2048
BATCH = 1
SEQ = 16384
DEPTH = 4

ATT_QK_DIM = 64
ATT_V_DIM = 2 * ATT_QK_DIM
ATT_WIDTH = D_MODEL // 2
ATT_HEADS = ATT_WIDTH // ATT_V_DIM
Q_BLOCK = 128
ROPE_THETA = 10000.0

HGRN_WIDTH = D_MODEL - ATT_WIDTH
HGRN_HEADS = 8
HGRN_V_DIM = HGRN_WIDTH // HGRN_HEADS
HGRN_K_DIM = 128
HGRN_F_WIDTH = HGRN_HEADS * HGRN_K_DIM
CHUNK = 64

N_EXPERTS = 16
CAPACITY_FACTOR = 2
EXPERT_FF = D_MODEL // 2

EPS = 1e-6

COL_WIDTHS = (
    ATT_HEADS * 2 * ATT_QK_DIM,
    ATT_HEADS * 2 * ATT_QK_DIM,
    ATT_WIDTH,
    HGRN_F_WIDTH,
    HGRN_F_WIDTH,
    HGRN_F_WIDTH,
    HGRN_WIDTH,
    HGRN_WIDTH,
)
IN_COLS = sum(COL_WIDTHS)
SPLIT_IDX = tuple(int(v) for v in np.cumsum(COL_WIDTHS)[:-1])

kernel_name = "hybrid_diffattn_hgrn2_ecmoe_encoder"


def rmsnorm(x, g):
    xf = x.astype(jnp.float32)
    y = xf * lax.rsqrt(jnp.mean(xf * xf, axis=-1, keepdims=True) + EPS)
    return (y * g.astype(jnp.float32)).astype(x.dtype)


def rope_tables(positions):
    inv = ROPE_THETA ** (-jnp.arange(0, ATT_QK_DIM, 2, dtype=jnp.float32) / ATT_QK_DIM)
    ang = positions.astype(jnp.float32)[..., None] * inv
    return jnp.cos(ang), jnp.sin(ang)


def apply_rope(x, cos, sin):
    xf = x.astype(jnp.float32)
    x1, x2 = jnp.split(xf, 2, axis=-1)
    c = cos[:, :, None, None, :]
    s = sin[:, :, None, None, :]
    return jnp.concatenate([x1 * c - x2 * s, x2 * c + x1 * s], axis=-1).astype(x.dtype)


def diff_attention(q, k, v, lam, sub_g, lam_init):
    B, T = q.shape[0], q.shape[1]
    nb = T // Q_BLOCK
    qb = (q * (ATT_QK_DIM ** -0.5)).reshape(B, nb, Q_BLOCK, ATT_HEADS, 2, ATT_QK_DIM)
    qb = qb.transpose(1, 0, 2, 3, 4, 5)

    def block(qi):
        s = jnp.einsum('bqhmd,bkhmd->bhmqk', qi, k).astype(jnp.float32)
        p = jax.nn.softmax(s, axis=-1)
        w = p[:, :, 0] - lam * p[:, :, 1]
        return jnp.einsum('bhqk,bkhe->bqhe', w.astype(v.dtype), v)

    o = lax.map(block, qb)
    o = o.transpose(1, 0, 2, 3, 4).reshape(B, T, ATT_HEADS, ATT_V_DIM)
    o = rmsnorm(o, sub_g) * (1.0 - lam_init)
    return o.reshape(B, T, ATT_WIDTH)


def gated_linear_scan(q, k, v, logf):
    B, T, H, dk = q.shape
    dv = v.shape[-1]
    n = T // CHUNK

    def to_chunks(a):
        return a.reshape(B, n, CHUNK, H, a.shape[-1]).transpose(1, 0, 3, 2, 4)

    mask = jnp.tril(jnp.ones((CHUNK, CHUNK), dtype=bool))[:, :, None]

    def step(S, xs):
        qc, kc, vc, lc = xs
        b = jnp.cumsum(lc, axis=2)
        inter = jnp.einsum('bhtd,bhde->bhte', qc * jnp.exp(b), S)
        diff = b[:, :, :, None, :] - b[:, :, None, :, :]
        dec = jnp.exp(jnp.where(mask, diff, -jnp.inf))
        A = jnp.einsum('bhtd,bhtsd,bhsd->bhts', qc, dec, kc)
        o = inter + jnp.einsum('bhts,bhse->bhte', A, vc)
        b_last = b[:, :, -1:, :]
        S_new = jnp.exp(b_last[:, :, 0, :])[..., None] * S + jnp.einsum(
            'bhsd,bhse->bhde', kc * jnp.exp(b_last - b), vc)
        return S_new, o

    S0 = jnp.zeros((B, H, dk, dv), jnp.float32)
    _, o = lax.scan(step, S0, (to_chunks(q), to_chunks(k), to_chunks(v), to_chunks(logf)))
    return o.transpose(1, 0, 3, 2, 4).reshape(B, T, H, dv)


def hgrn2_bidirectional(q, z_fwd, z_bwd, i, g, lb_fwd, lb_bwd, norm_g):
    B, T = q.shape[0], q.shape[1]
    dt = i.dtype

    def gates(z, lb):
        zf = z.astype(jnp.float32)
        f = lb + (1.0 - lb) * jax.nn.sigmoid(zf)
        logf = jnp.log(f)
        kk = (1.0 - lb) * jax.nn.sigmoid(-zf)
        return (logf.reshape(B, T, HGRN_HEADS, HGRN_K_DIM),
                kk.reshape(B, T, HGRN_HEADS, HGRN_K_DIM))

    qh = (jax.nn.silu(q.astype(jnp.float32)) * (HGRN_K_DIM ** -0.5)).reshape(
        B, T, HGRN_HEADS, HGRN_K_DIM)
    vh = i.astype(jnp.float32).reshape(B, T, HGRN_HEADS, HGRN_V_DIM)
    lf_f, k_f = gates(z_fwd, lb_fwd)
    lf_b, k_b = gates(z_bwd, lb_bwd)
    flip = lambda a: jnp.flip(a, axis=1)
    Q = jnp.concatenate([qh, flip(qh)], axis=2)
    K = jnp.concatenate([k_f, flip(k_b)], axis=2)
    LF = jnp.concatenate([lf_f, flip(lf_b)], axis=2)
    V = jnp.concatenate([vh, flip(vh)], axis=2)
    o = gated_linear_scan(Q, K, V, LF)
    o = o[:, :, :HGRN_HEADS] + flip(o[:, :, HGRN_HEADS:])
    o = rmsnorm(o, norm_g) * jax.nn.silu(
        g.astype(jnp.float32).reshape(B, T, HGRN_HEADS, HGRN_V_DIM))
    return o.reshape(B, T, HGRN_WIDTH).astype(dt)


def expert_choice_ffn(x, router_w, w_gate, w_up, w_down):
    B, T, D = x.shape
    cap = CAPACITY_FACTOR * T // N_EXPERTS
    logits = jnp.einsum('btd,de->bte', x, router_w).astype(jnp.float32)
    aff = jax.nn.softmax(logits, axis=-1)
    gate, idx = lax.top_k(aff.transpose(0, 2, 1), cap)
    xg = jax.vmap(lambda xb, ib: xb[ib])(x, idx)
    h = jax.nn.silu(jnp.einsum('becd,edf->becf', xg, w_gate)) * jnp.einsum(
        'becd,edf->becf', xg, w_up)
    y = jnp.einsum('becf,efd->becd', h, w_down) * gate[..., None].astype(x.dtype)
    flat = (idx + (jnp.arange(B, dtype=jnp.int32) * T)[:, None, None]).reshape(-1)
    out = jax.ops.segment_sum(y.reshape(-1, D), flat, num_segments=B * T)
    return out.reshape(B, T, D).astype(x.dtype)


def setup_inputs(seed: int = 0) -> dict:
    key = jax.random.key(seed)
    ks = jax.random.split(key, 16)
    f32 = jnp.float32
    x = jax.random.normal(ks[0], (BATCH, SEQ, D_MODEL), f32)
    positions = jnp.broadcast_to(jnp.arange(SEQ, dtype=jnp.int32), (BATCH, SEQ))
    norm1_g = 1.0 + 0.02 * jax.random.normal(ks[1], (DEPTH, D_MODEL), f32)
    w_in = jax.random.normal(ks[2], (DEPTH, D_MODEL, IN_COLS), f32) * D_MODEL ** -0.5
    diff_lambda = 0.1 * jax.random.normal(ks[3], (DEPTH, 4, ATT_QK_DIM), f32)
    diff_subln_g = 1.0 + 0.02 * jax.random.normal(ks[4], (DEPTH, ATT_V_DIM), f32)
    hgrn_lb_logits = 0.5 * jax.random.normal(ks[5], (2, DEPTH, HGRN_F_WIDTH), f32)
    hgrn_norm_g = 1.0 + 0.02 * jax.random.normal(ks[6], (DEPTH, HGRN_V_DIM), f32)
    w_out = jax.random.normal(ks[7], (DEPTH, ATT_WIDTH + HGRN_WIDTH, D_MODEL), f32) * (
        (ATT_WIDTH + HGRN_WIDTH) ** -0.5)
    norm2_g = 1.0 + 0.02 * jax.random.normal(ks[8], (DEPTH, D_MODEL), f32)
    router_w = jax.random.normal(ks[9], (DEPTH, D_MODEL, N_EXPERTS), f32) * D_MODEL ** -0.5
    expert_w_gate = jax.random.normal(
        ks[10], (DEPTH, N_EXPERTS, D_MODEL, EXPERT_FF), f32) * D_MODEL ** -0.5
    expert_w_up = jax.random.normal(
        ks[11], (DEPTH, N_EXPERTS, D_MODEL, EXPERT_FF), f32) * D_MODEL ** -0.5
    expert_w_down = jax.random.normal(
        ks[12], (DEPTH, N_EXPERTS, EXPERT_FF, D_MODEL), f32) * EXPERT_FF ** -0.5
    final_norm_g = 1.0 + 0.02 * jax.random.normal(ks[13], (D_MODEL,), f32)
    return {"x": x, "positions": positions, "norm1_g": norm1_g, "w_in": w_in,
            "diff_lambda": diff_lambda, "diff_subln_g": diff_subln_g,
            "hgrn_lb_logits": hgrn_lb_logits, "hgrn_norm_g": hgrn_norm_g,
            "w_out": w_out, "norm2_g": norm2_g, "router_w": router_w,
            "expert_w_gate": expert_w_gate, "expert_w_up": expert_w_up,
            "expert_w_down": expert_w_down, "final_norm_g": final_norm_g}


def reference(x, positions, norm1_g, w_in, diff_lambda, diff_subln_g, hgrn_lb_logits,
              hgrn_norm_g, w_out, norm2_g, router_w, expert_w_gate, expert_w_up,
              expert_w_down, final_norm_g):
    B, T, _ = x.shape
    cos, sin = rope_tables(positions)
    lb_sm = jax.nn.softmax(hgrn_lb_logits.astype(jnp.float32), axis=1)
    lbs = jnp.cumsum(lb_sm, axis=1) - lb_sm[:, :1]
    for l in range(DEPTH):
        h = rmsnorm(x, norm1_g[l])
        proj = jnp.einsum('btd,dc->btc', h, w_in[l])
        aq, ak, av, hq, hzf, hzb, hi, hg = jnp.split(proj, SPLIT_IDX, axis=-1)
        aq = apply_rope(aq.reshape(B, T, ATT_HEADS, 2, ATT_QK_DIM), cos, sin)
        ak = apply_rope(ak.reshape(B, T, ATT_HEADS, 2, ATT_QK_DIM), cos, sin)
        av = av.reshape(B, T, ATT_HEADS, ATT_V_DIM)
        lam_init = 0.8 - 0.6 * math.exp(-0.3 * l)
        lv = diff_lambda[l].astype(jnp.float32)
        lam = jnp.exp(jnp.sum(lv[0] * lv[1])) - jnp.exp(jnp.sum(lv[2] * lv[3])) + lam_init
        att_out = diff_attention(aq, ak, av, lam, diff_subln_g[l], lam_init)
        hgrn_out = hgrn2_bidirectional(hq, hzf, hzb, hi, hg, lbs[0, l], lbs[1, l],
                                       hgrn_norm_g[l])
        mixed = jnp.concatenate([att_out, hgrn_out.astype(att_out.dtype)], axis=-1)
        x = x + jnp.einsum('btc,cd->btd', mixed, w_out[l])
        x = x + expert_choice_ffn(rmsnorm(x, norm2_g[l]), router_w[l], expert_w_gate[l],
                                  expert_w_up[l], expert_w_down[l])
    return rmsnorm(x, final_norm_g)
```

```python
import math
from contextlib import ExitStack
import numpy as np
import concourse.bass as bass
import concourse.mybir as mybir
from concourse.bass_utils import run_bass_kernel_spmd

F32 = mybir.dt.float32
BF16 = mybir.dt.bfloat16
I32 = mybir.dt.int32
AF = mybir.ActivationFunctionType
ALU = mybir.AluOpType
AX = mybir.AxisListType

D = 2048
KC = 16
NCORES = 8
EPS = 1e-6
TT = 512
NEXP = 16
FF = 1024
PI = math.pi


class Buf:
    __slots__ = ("w", "r")

    def __init__(self):
        self.w = {}
        self.r = {}


class Op:
    __slots__ = ("eng", "fn", "deps", "sig", "sem", "val", "inc", "cover", "key")


class Rec:
    ENGS = ("pe", "act", "dve", "pool", "sp")

    def __init__(self, nc, es):
        self.nc = nc
        self.es = es
        self.q = {e: [] for e in self.ENGS}
        self.esem = {e: es.enter_context(nc.semaphore("sem_" + e)) for e in ("pe", "act", "dve", "pool")}
        self.ecnt = {e: 0 for e in self.esem}
        self.chan = {}
        self.pending = {e: [] for e in self.ENGS}
        self.waited = {e: {} for e in self.ENGS}
        self.last = {}

    def _chan(self, name):
        if name not in self.chan:
            self.chan[name] = [self.es.enter_context(self.nc.semaphore("dma_" + name)), 0]
        return self.chan[name]

    def _deps(self, op, reads, writes):
        deps = {}
        for b in reads:
            for k, o in b.w.items():
                deps[id(o)] = o
        for b in writes:
            for k, o in b.w.items():
                deps[id(o)] = o
            for k, o in b.r.items():
                deps[id(o)] = o
        op.deps = [o for o in deps.values() if not (o.eng == "pe" and op.eng == "pe" and o.key == "pe")]
        for b in reads:
            b.r[op.key] = op
        for b in writes:
            b.w[op.key] = op
            b.r = {}

    def op(self, eng, fn, reads=(), writes=(), nosig=False):
        o = Op()
        o.eng, o.fn, o.sig, o.cover, o.key = eng, fn, not nosig, None, eng
        o.sem, o.val, o.inc = self.esem[eng], None, 1
        self._deps(o, reads, writes)
        self.q[eng].append(o)
        if nosig:
            self.pending[eng].append(o)
        else:
            for p in self.pending[eng]:
                p.cover = o
            self.pending[eng] = []
        self.last[eng] = o
        return o

    def dma(self, q, out, in_, reads=(), writes=(), chan=None, **kw):
        c = self._chan(chan)
        c[1] += 16
        o = Op()
        o.eng, o.sig, o.cover, o.key = q, True, None, "dma_" + chan
        o.sem, o.val, o.inc = c[0], c[1], 16
        o.fn = lambda e: e.dma_start(out=out, in_=in_, **kw)
        self._deps(o, reads, writes)
        self.q[q].append(o)
        self.last["dma_" + chan] = o
        return o

    def barrier(self):
        allb = Buf()
        for o in list(self.last.values()):
            allb.w[id(o)] = o
        for e in self.ENGS:
            o = Op()
            o.eng, o.fn, o.sig, o.cover, o.key = e, None, False, None, e
            o.sem, o.val, o.inc = None, None, 0
            o.deps = list(allb.w.values())
            self.q[e].append(o)

    def flush(self, final=False):
        self.barrier()
        for e in self.esem:
            for p in self.pending[e]:
                p.sig = True
            self.pending[e] = []
            for o in self.q[e]:
                if o.fn is not None and o.sig and o.val is None:
                    self.ecnt[e] += 1
                    o.val = self.ecnt[e]
        with self.nc.Block() as block:
            meth = {"pe": block.tensor, "act": block.scalar, "dve": block.vector, "pool": block.gpsimd, "sp": block.sync}
            for e in self.ENGS:
                ops = self.q[e]
                wd = self.waited[e]

                def body(h, ops=ops, wd=wd):
                    for o in ops:
                        need = {}
                        for d in o.deps:
                            t = d if d.sig else d.cover
                            while t is not None and not t.sig:
                                t = t.cover
                            if t is None:
                                raise RuntimeError("unsignalled dep")
                            k = id(t.sem)
                            if k not in need or need[k][1] < t.val:
                                need[k] = (t.sem, t.val)
                        for k, (s, v) in need.items():
                            if wd.get(k, 0) < v:
                                h.wait_ge(s, v)
                                wd[k] = v
                        if o.fn is not None:
                            ins = o.fn(h)
                            if o.sig:
                                ins.then_inc(o.sem, o.inc)

                meth[e](body)
        self.q = {e: [] for e in self.ENGS}


def _mk(nc):
    es = ExitStack()
    return es, Rec(nc, es)


def _rmsnorm_rstd(R, xt, sq, ssq, ps, rstd, sd, ones_f, cst, B):
    R.op("act", lambda e: e.activation(out=sq[:], in_=xt, func=AF.Square), reads=[B["xt"]], writes=[B["sq"]])
    R.op("dve", lambda e: e.tensor_reduce(out=ssq[:], in_=sq[:].rearrange("p (k t) -> p t k", k=KC), axis=AX.X, op=ALU.add),
         reads=[B["sq"]], writes=[B["ssq"]])
    R.op("pe", lambda e: e.matmul(ps, lhsT=ones_f[:], rhs=ssq[:], start=True, stop=True), reads=[B["ssq"]], writes=[B["ps_r"]])
    R.op("act", lambda e: e.activation(out=sd[:], in_=ps, func=AF.Sqrt, scale=1.0 / D, bias=cst[:, 0:1]),
         reads=[B["ps_r"]], writes=[B["sd"]])
    R.op("dve", lambda e: e.reciprocal(out=rstd[:], in_=sd[:]), reads=[B["sd"]], writes=[B["rstd"]])


def build_C(TL):
    nc = bass.Bass("TRN2", target_bir_lowering=False)
    xT = nc.dram_tensor("xT", [D, TL], F32, kind="ExternalInput").ap()
    mT = nc.dram_tensor("mT", [D, TL], F32, kind="ExternalInput").ap()
    wo = nc.dram_tensor("wo", [D, D], F32, kind="ExternalInput").ap()
    g2 = nc.dram_tensor("g2", [128, KC], F32, kind="ExternalInput").ap()
    rw = nc.dram_tensor("rw", [D, NEXP], F32, kind="ExternalInput").ap()
    cst_d = nc.dram_tensor("cst", [128, 8], F32, kind="ExternalInput").ap()
    x1T = nc.dram_tensor("x1T", [D, TL], F32, kind="ExternalOutput").ap()
    aff = nc.dram_tensor("aff", [TL, NEXP], F32, kind="ExternalOutput").ap()
    es, R = _mk(nc)
    with es:
        sb = lambda n, s, d: es.enter_context(nc.sbuf_tensor("s_" + n, s, d))
        Wo = sb("Wo", [128, KC, D], BF16)
        g2s = sb("g2s", [128, KC], F32)
        rws = sb("rws", [128, KC, NEXP], F32)
        cst = sb("cst", [128, 8], F32)
        ones_f = sb("ones_f", [128, 128], F32)
        xt = [sb("xt%d" % i, [128, KC * TT], F32) for i in range(1)]
        mt = [sb("mt%d" % i, [128, KC, TT], BF16) for i in range(1)]
        sq = sb("sq", [128, KC * TT], F32)
        h2 = sb("h2", [128, KC, TT], F32)
        ssq = sb("ssq", [128, TT], F32)
        sd = sb("sd", [128, TT], F32)
        rstd = sb("rstd", [128, TT], F32)
        lg = sb("lg", [128, NEXP], F32)
        ex = sb("ex", [128, NEXP], F32)
        af = [sb("af%d" % i, [128, NEXP], F32) for i in range(2)]
        mx = sb("mx", [128, 4], F32)
        ps = [es.enter_context(nc.psum_tensor("ps%d" % i, [128, TT], F32)) for i in range(8)]
        B = {k: Buf() for k in ["Wo", "g2s", "rws", "cst", "ones", "sq", "h2", "ssq", "sd", "rstd", "lg", "ex", "mx", "ps_r", "xt"]}
        Bxt = [Buf(), Buf()]
        Bmt = [Buf(), Buf()]
        Baf = [Buf(), Buf()]
        Bps = [Buf() for _ in range(8)]
        R.op("dve", lambda e: e.memset(ones_f[:], 1.0), writes=[B["ones"]])
        for kc in range(KC):
            R.dma("pool", Wo[:, kc, :], wo[kc * 128:(kc + 1) * 128, :], writes=[B["Wo"]], chan="wo")
        R.dma("sp", g2s[:], g2, writes=[B["g2s"]], chan="c0")
        R.dma("sp", cst[:], cst_d, writes=[B["cst"]], chan="c0")
        R.dma("sp", rws[:], rw.rearrange("(k p) e -> p k e", p=128), writes=[B["rws"]], chan="c0")
        xv = xT.rearrange("(k p) t -> p k t", p=128)
        mv = mT.rearrange("(k p) t -> p k t", p=128)
        x1v = x1T.rearrange("(k p) t -> p k t", p=128)
        for ti in range(TL // TT):
            b = 0
            tsl = slice(ti * TT, (ti + 1) * TT)
            xt3 = xt[b][:].rearrange("p (k t) -> p k t", k=KC)
            R.dma("sp", xt3, xv[:, :, tsl], writes=[Bxt[b]], chan="x%d" % b)
            R.dma("pool", mt[b][:], mv[:, :, tsl], writes=[Bmt[b]], chan="m%d" % b)
            for dc in range(KC):
                pb = dc % 4
                for kc in range(KC):
                    R.op("pe", lambda e, kc=kc, dc=dc, pb=pb, b=b: e.matmul(
                        ps[pb][:], lhsT=Wo[:, kc, dc * 128:(dc + 1) * 128], rhs=mt[b][:, kc, :],
                        start=(kc == 0), stop=(kc == KC - 1)),
                        reads=[B["Wo"], Bmt[b]], writes=[Bps[pb]], nosig=(kc < KC - 1))
                R.op("dve", lambda e, dc=dc, pb=pb, b=b: e.tensor_tensor(
                    out=xt[b][:, dc * TT:(dc + 1) * TT], in0=ps[pb][:], in1=xt[b][:, dc * TT:(dc + 1) * TT], op=ALU.add),
                    reads=[Bps[pb], Bxt[b]], writes=[Bxt[b]])
            R.dma("sp", x1v[:, :, tsl], xt3, reads=[Bxt[b]], chan="xo%d" % b)
            Bn = dict(B)
            Bn["xt"] = Bxt[b]
            _rmsnorm_rstd(R, xt[b][:], sq, ssq, ps[4][:], rstd, sd, ones_f, cst, {**Bn, "ps_r": Bps[4]})
            for kc in range(KC):
                R.op("dve", lambda e, kc=kc, b=b: e.scalar_tensor_tensor(
                    out=h2[:, kc, :], in0=xt[b][:, kc * TT:(kc + 1) * TT], scalar=g2s[:, kc:kc + 1], in1=rstd[:],
                    op0=ALU.mult, op1=ALU.mult), reads=[Bxt[b], B["g2s"], B["rstd"]], writes=[B["h2"]])
            for ts in range(TT // 128):
                pb = 5 + ts % 2
                for kc in range(KC):
                    R.op("pe", lambda e, kc=kc, ts=ts, pb=pb: e.matmul(
                        ps[pb][:, 0:NEXP], lhsT=h2[:, kc, ts * 128:(ts + 1) * 128], rhs=rws[:, kc, :],
                        start=(kc == 0), stop=(kc == KC - 1)), reads=[B["h2"], B["rws"]], writes=[Bps[pb]],
                        nosig=(kc < KC - 1))
                ab = ts % 2
                R.op("dve", lambda e, pb=pb: e.tensor_reduce(out=mx[:, 0:1], in_=ps[pb][:, 0:NEXP], axis=AX.X, op=ALU.max),
                     reads=[Bps[pb]], writes=[B["mx"]])
                R.op("dve", lambda e: e.tensor_scalar(out=mx[:, 1:2], in0=mx[:, 0:1], scalar1=-1.0, scalar2=None, op0=ALU.mult),
                     reads=[B["mx"]], writes=[B["mx"]])
                R.op("act", lambda e, pb=pb: e.activation(out=ex[:], in_=ps[pb][:, 0:NEXP], func=AF.Exp, bias=mx[:, 1:2],
                                                          scale=1.0, accum_out=mx[:, 2:3]),
                     reads=[Bps[pb], B["mx"]], writes=[B["ex"], B["mx"]])
                R.op("dve", lambda e: e.reciprocal(out=mx[:, 3:4], in_=mx[:, 2:3]), reads=[B["mx"]], writes=[B["mx"]])
                R.op("dve", lambda e, ab=ab: e.tensor_scalar(out=af[ab][:], in0=ex[:], scalar1=mx[:, 3:4], scalar2=None, op0=ALU.mult),
                     reads=[B["ex"], B["mx"]], writes=[Baf[ab]])
                r0 = ti * TT + ts * 128
                R.dma("sp", aff[r0:r0 + 128, :], af[ab][:], reads=[Baf[ab]], chan="af%d" % ab)
        R.flush()
    return nc


def build_D(TL, T):
    SEG = T // 8
    CAP = float(2 * T // NEXP)
    nc = bass.Bass("TRN2", target_bir_lowering=False)
    xT = nc.dram_tensor("xT", [D, TL], F32, kind="ExternalInput").ap()
    g2 = nc.dram_tensor("g2", [128, KC], F32, kind="ExternalInput").ap()
    gf = nc.dram_tensor("gf", [128, KC], F32, kind="ExternalInput").ap()
    afa = nc.dram_tensor("afa", [128, SEG], F32, kind="ExternalInput").ap()
    afl = nc.dram_tensor("afl", [NEXP, TL], F32, kind="ExternalInput").ap()
    wg = nc.dram_tensor("wg", [NEXP, D, FF], F32, kind="ExternalInput").ap()
    wu = nc.dram_tensor("wu", [NEXP, D, FF], F32, kind="ExternalInput").ap()
    wd = nc.dram_tensor("wd", [NEXP, FF, D], F32, kind="ExternalInput").ap()
    cst_d = nc.dram_tensor("cst", [128, 8], F32, kind="ExternalInput").ap()
    gm_d = nc.dram_tensor("gm", [128, 128], F32, kind="ExternalInput").ap()
    sel_d = nc.dram_tensor("sel", [128, NEXP], F32, kind="ExternalInput").ap()
    x2T = nc.dram_tensor("x2T", [D, TL], F32, kind="ExternalOutput").ap()
    yT = nc.dram_tensor("yT", [D, TL], F32, kind="ExternalOutput").ap()
    es, R = _mk(nc)
    with es:
        sb = lambda n, s, d: es.enter_context(nc.sbuf_tensor("s_" + n, s, d))
        cst = sb("cst", [128, 8], F32)
        g2s = sb("g2s", [128, KC], F32)
        gfs = sb("gfs", [128, KC], F32)
        gm = sb("gm", [128, 128], F32)
        sel = sb("sel", [128, NEXP], F32)
        ones_f = sb("ones_f", [128, 128], F32)
        AFA = sb("AFA", [128, SEG], F32)
        junk = sb("junk", [128, SEG], F32)
        bs = sb("bs", [128, 16], F32)
        thr_r = sb("thr_r", [128, NEXP], F32)
        thr = sb("thr", [128, NEXP], F32)
        wgtB = sb("wgtB", [128, NEXP, TT], F32)
        arow = [sb("arow%d" % i, [128, TT], F32) for i in range(2)]
        xt = sb("xt", [128, KC * TT], F32)
        sq = sb("sq", [128, KC * TT], F32)
        xn = sb("xn", [128, KC, TT], BF16)
        ssq = sb("ssq", [128, TT], F32)
        sd = sb("sd", [128, TT], F32)
        rstd = sb("rstd", [128, TT], F32)
        WG = [sb("WG%d" % i, [128, KC, 256], BF16) for i in range(2)]
        WU = [sb("WU%d" % i, [128, KC, 256], BF16) for i in range(2)]
        WD = [sb("WD%d" % i, [128, 2, D], BF16) for i in range(2)]
        sg = [sb("sg%d" % i, [128, TT], F32) for i in range(2)]
        uw = [sb("uw%d" % i, [128, TT], F32) for i in range(2)]
        hT = [sb("hT%d" % i, [128, 2, TT], BF16) for i in range(2)]
        ps = [es.enter_context(nc.psum_tensor("ps%d" % i, [128, TT], F32)) for i in range(8)]
        names = ["cst", "g2s", "gfs", "gm", "sel", "ones", "AFA", "junk", "bs", "thr_r", "thr", "wgtB", "xt", "sq", "xn",
                 "ssq", "sd", "rstd"]
        B = {k: Buf() for k in names}
        Bps = [Buf() for _ in range(8)]
        Bar = [Buf(), Buf()]
        BW = [Buf(), Buf()]
        Bsg = [Buf(), Buf()]
        Buw = [Buf(), Buf()]
        BhT = [[Buf() for _ in range(4)] for _ in range(2)]
        R.op("dve", lambda e: e.memset(ones_f[:], 1.0), writes=[B["ones"]])
        R.dma("sp", cst[:], cst_d, writes=[B["cst"]], chan="c0")
        R.dma("sp", g2s[:], g2, writes=[B["g2s"]], chan="c0")
        R.dma("sp", gfs[:], gf, writes=[B["gfs"]], chan="c0")
        R.dma("sp", gm[:], gm_d, writes=[B["gm"]], chan="c0")
        R.dma("sp", sel[:], sel_d, writes=[B["sel"]], chan="c0")
        R.dma("sp", AFA[:], afa, writes=[B["AFA"]], chan="c0")
        R.op("dve", lambda e: e.memset(bs[:, 0:1], 0.0), writes=[B["bs"]])
        R.op("dve", lambda e: e.memset(bs[:, 1:2], 2.0), writes=[B["bs"]])
        for it in range(40):
            R.op("dve", lambda e: e.tensor_tensor(out=bs[:, 2:3], in0=bs[:, 0:1], in1=bs[:, 1:2], op=ALU.add),
                 reads=[B["bs"]], writes=[B["bs"]])
            R.op("dve", lambda e: e.tensor_scalar(out=bs[:, 2:3], in0=bs[:, 2:3], scalar1=0.5, scalar2=None, op0=ALU.mult),
                 reads=[B["bs"]], writes=[B["bs"]])
            R.op("dve", lambda e: e.tensor_scalar(out=junk[:], in0=AFA[:], scalar1=bs[:, 2:3], scalar2=0.0, op0=ALU.is_ge,
                                                  op1=ALU.add, accum_out=bs[:, 3:4]),
                 reads=[B["AFA"], B["bs"]], writes=[B["junk"], B["bs"]])
            R.op("pe", lambda e: e.matmul(ps[0][:, 0:1], lhsT=gm[:], rhs=bs[:, 3:4], start=True, stop=True),
                 reads=[B["gm"], B["bs"]], writes=[Bps[0]])
            R.op("dve", lambda e: e.tensor_scalar(out=bs[:, 4:5], in0=ps[0][:, 0:1], scalar1=CAP, scalar2=None, op0=ALU.is_ge),
                 reads=[Bps[0]], writes=[B["bs"]])
            R.op("dve", lambda e: e.tensor_tensor(out=bs[:, 5:6], in0=bs[:, 2:3], in1=bs[:, 4:5], op=ALU.mult),
                 reads=[B["bs"]], writes=[B["bs"]])
            R.op("dve", lambda e: e.tensor_tensor(out=bs[:, 0:1], in0=bs[:, 0:1], in1=bs[:, 5:6], op=ALU.max),
                 reads=[B["bs"]], writes=[B["bs"]])
            R.op("dve", lambda e: e.scalar_tensor_tensor(out=bs[:, 6:7], in0=bs[:, 4:5], scalar=4.0, in1=bs[:, 2:3],
                                                         op0=ALU.mult, op1=ALU.add), reads=[B["bs"]], writes=[B["bs"]])
            R.op("dve", lambda e: e.tensor_tensor(out=bs[:, 1:2], in0=bs[:, 1:2], in1=bs[:, 6:7], op=ALU.min),
                 reads=[B["bs"]], writes=[B["bs"]])
        R.op("dve", lambda e: e.tensor_scalar(out=thr_r[:], in0=sel[:], scalar1=bs[:, 0:1], scalar2=None, op0=ALU.mult),
             reads=[B["sel"], B["bs"]], writes=[B["thr_r"]])
        R.op("pe", lambda e: e.matmul(ps[1][:, 0:NEXP], lhsT=ones_f[:], rhs=thr_r[:], start=True, stop=True),
             reads=[B["ones"], B["thr_r"]], writes=[Bps[1]])
        R.op("dve", lambda e: e.tensor_copy(out=thr[:], in_=ps[1][:, 0:NEXP]), reads=[Bps[1]], writes=[B["thr"]])
        xv = xT.rearrange("(k p) t -> p k t", p=128)
        x2v = x2T.rearrange("(k p) t -> p k t", p=128)
        yv = yT.rearrange("(k p) t -> p k t", p=128)
        wstep = 0
        for ti in range(TL // TT):
            tsl = slice(ti * TT, (ti + 1) * TT)
            xt3 = xt[:].rearrange("p (k t) -> p k t", k=KC)
            R.dma("sp", xt3, xv[:, :, tsl], writes=[B["xt"]], chan="x")
            for ex_ in range(NEXP):
                ab = ex_ % 2
                R.dma("sp", arow[ab][:], afl[ex_:ex_ + 1, tsl].partition_broadcast(128), writes=[Bar[ab]], chan="ar%d" % ab)
                R.op("dve", lambda e, ex_=ex_, ab=ab: e.scalar_tensor_tensor(
                    out=wgtB[:, ex_, :], in0=arow[ab][:], scalar=thr[:, ex_:ex_ + 1], in1=arow[ab][:],
                    op0=ALU.is_ge, op1=ALU.mult), reads=[Bar[ab], B["thr"]], writes=[B["wgtB"]])
            _rmsnorm_rstd(R, xt[:], sq, ssq, ps[0][:], rstd, sd, ones_f, cst, {**B, "ps_r": Bps[0]})
            for kc in range(KC):
                R.op("dve", lambda e, kc=kc: e.scalar_tensor_tensor(
                    out=xn[:, kc, :], in0=xt[:, kc * TT:(kc + 1) * TT], scalar=g2s[:, kc:kc + 1], in1=rstd[:],
                    op0=ALU.mult, op1=ALU.mult), reads=[B["xt"], B["g2s"], B["rstd"]], writes=[B["xn"]])
            for ex_ in range(NEXP):
                for fh in range(4):
                    wb = wstep % 2
                    wstep += 1
                    fsl = slice(fh * 256, (fh + 1) * 256)
                    for kc in range(KC):
                        R.dma("pool", WG[wb][:, kc, :], wg[ex_, kc * 128:(kc + 1) * 128, fsl], writes=[BW[wb]], chan="w%d" % wb)
                        R.dma("pool", WU[wb][:, kc, :], wu[ex_, kc * 128:(kc + 1) * 128, fsl], writes=[BW[wb]], chan="w%d" % wb)
                    for fc in range(2):
                        R.dma("pool", WD[wb][:, fc, :], wd[ex_, fh * 256 + fc * 128: fh * 256 + (fc + 1) * 128, :],
                              writes=[BW[wb]], chan="w%d" % wb)
                    hb = wb
                    for fc in range(2):
                        pg, pu = 2 * (fc % 2), 2 * (fc % 2) + 1
                        for kc in range(KC):
                            R.op("pe", lambda e, kc=kc, fc=fc, wb=wb, pg=pg: e.matmul(
                                ps[pg][:], lhsT=WG[wb][:, kc, fc * 128:(fc + 1) * 128], rhs=xn[:, kc, :],
                                start=(kc == 0), stop=(kc == KC - 1)), reads=[BW[wb], B["xn"]], writes=[Bps[pg]],
                                nosig=(kc < KC - 1))
                        for kc in range(KC):
                            R.op("pe", lambda e, kc=kc, fc=fc, wb=wb, pu=pu: e.matmul(
                                ps[pu][:], lhsT=WU[wb][:, kc, fc * 128:(fc + 1) * 128], rhs=xn[:, kc, :],
                                start=(kc == 0), stop=(kc == KC - 1)), reads=[BW[wb], B["xn"]], writes=[Bps[pu]],
                                nosig=(kc < KC - 1))
                        sb_ = fc % 2
                        R.op("act", lambda e, pg=pg, sb_=sb_: e.activation(out=sg[sb_][:], in_=ps[pg][:], func=AF.Silu),
                             reads=[Bps[pg]], writes=[Bsg[sb_]])
                        R.op("dve", lambda e, pu=pu, sb_=sb_, ex_=ex_: e.tensor_tensor(
                            out=uw[sb_][:], in0=ps[pu][:], in1=wgtB[:, ex_, :], op=ALU.mult),
                            reads=[Bps[pu], B["wgtB"]], writes=[Buw[sb_]])
                        R.op("pool", lambda e, sb_=sb_, hb=hb, fc=fc: e.tensor_tensor(
                            out=hT[hb][:, fc, :], in0=sg[sb_][:], in1=uw[sb_][:], op=ALU.mult),
                            reads=[Bsg[sb_], Buw[sb_]], writes=[BhT[hb][fc]])
                    for dc in range(KC):
                        pb = 4 + dc % 4
                        for fc in range(2):
                            R.op("pe", lambda e, fc=fc, dc=dc, wb=wb, hb=hb, pb=pb: e.matmul(
                                ps[pb][:], lhsT=WD[wb][:, fc, dc * 128:(dc + 1) * 128], rhs=hT[hb][:, fc, :],
                                start=(fc == 0), stop=(fc == 1)), reads=[BW[wb], BhT[hb][fc]], writes=[Bps[pb]],
                                nosig=(fc < 1))
                        R.op("dve", lambda e, dc=dc, pb=pb: e.tensor_tensor(
                            out=xt[:, dc * TT:(dc + 1) * TT], in0=ps[pb][:], in1=xt[:, dc * TT:(dc + 1) * TT], op=ALU.add),
                            reads=[Bps[pb], B["xt"]], writes=[B["xt"]])
            R.dma("sp", x2v[:, :, tsl], xt3, reads=[B["xt"]], chan="xo")
            _rmsnorm_rstd(R, xt[:], sq, ssq, ps[0][:], rstd, sd, ones_f, cst, {**B, "ps_r": Bps[0]})
            for kc in range(KC):
                R.op("dve", lambda e, kc=kc: e.scalar_tensor_tensor(
                    out=sq[:, kc * TT:(kc + 1) * TT], in0=xt[:, kc * TT:(kc + 1) * TT], scalar=gfs[:, kc:kc + 1], in1=rstd[:],
                    op0=ALU.mult, op1=ALU.mult), reads=[B["xt"], B["gfs"], B["rstd"]], writes=[B["sq"]])
            R.dma("sp", yv[:, :, tsl], sq[:].rearrange("p (k t) -> p k t", k=KC), reads=[B["sq"]], chan="yo")
        R.flush()
    return nc


def build_B(T):
    TN = T // TT
    NKC = T // 128
    nc = bass.Bass("TRN2", target_bir_lowering=False)
    xT = nc.dram_tensor("xT", [D, T], F32, kind="ExternalInput").ap()
    wB = nc.dram_tensor("wB", [D, 1280], F32, kind="ExternalInput").ap()
    g1 = nc.dram_tensor("g1", [128, KC], F32, kind="ExternalInput").ap()
    pos = nc.dram_tensor("pos", [1, T], I32, kind="ExternalInput").ap()
    dl_d = nc.dram_tensor("dl", [1, 256], F32, kind="ExternalInput").ap()
    cst_d = nc.dram_tensor("cst", [128, 16], F32, kind="ExternalInput").ap()
    lbl_d = nc.dram_tensor("lbl", [128, 8], F32, kind="ExternalInput").ap()
    tri_d = nc.dram_tensor("tri", [64, 128], F32, kind="ExternalInput").ap()
    cmask_d = nc.dram_tensor("cmask", [128, TT], F32, kind="ExternalInput").ap()
    ident_d = nc.dram_tensor("ident", [128, 128], F32, kind="ExternalInput").ap()
    mixT = nc.dram_tensor("mixT", [256, T], F32, kind="ExternalOutput").ap()
    QTd = nc.dram_tensor("QTd", [128, T], BF16).ap()
    KTd = nc.dram_tensor("KTd", [128, T], BF16).ap()
    Vd = nc.dram_tensor("Vd", [T, 128], BF16).ap()
    HId = nc.dram_tensor("HId", [T, 128], BF16).ap()
    QHd = nc.dram_tensor("QHd", [128, T], F32).ap()
    SFd = nc.dram_tensor("SFd", [128, T], F32).ap()
    SBd = nc.dram_tensor("SBd", [128, T], F32).ap()
    SGd = nc.dram_tensor("SGd", [128, T], F32).ap()
    OFd = nc.dram_tensor("OFd", [128, T], F32).ap()
    BD = {k: Buf() for k in ["QTd", "KTd", "Vd", "HId", "QHd", "SFd", "SBd", "SGd", "OFd"]}
    es0, R = _mk(nc)
    with es0:
        with ExitStack() as es:
            sb = lambda n, s, d: es.enter_context(nc.sbuf_tensor("a_" + n, s, d))
            W = sb("W", [128, KC, 1280], BF16)
            g1s = sb("g1s", [128, KC], F32)
            cst = sb("cst", [128, 16], F32)
            ones_f = sb("ones_f", [128, 128], F32)
            xt = sb("xt", [128, KC * TT], F32)
            sq = sb("sq", [128, KC * TT], F32)
            h = sb("h", [128, KC, TT], BF16)
            ssq = sb("ssq", [128, TT], F32)
            sd = sb("sd", [128, TT], F32)
            rstd = sb("rstd", [128, TT], F32)
            posi = sb("posi", [128, TT], I32)
            ang = sb("ang", [128, TT], F32)
            us = sb("us", [128, TT], F32)
            uc = sb("uc", [128, TT], F32)
            sinS = sb("sinS", [128, TT], F32)
            cosT = sb("cosT", [128, TT], F32)
            t1 = sb("t1", [128, TT], F32)
            t2 = sb("t2", [128, TT], F32)
            qk = [sb("qk%d" % i, [128, TT], BF16) for i in range(2)]
            gt = [sb("gt%d" % i, [128, TT], F32) for i in range(4)]
            vt = [sb("vt%d" % i, [128, 256], BF16) for i in range(2)]
            ps = [es.enter_context(nc.psum_tensor("aps%d" % i, [128, TT], F32)) for i in range(8)]
            B = {k: Buf() for k in ["W", "g1s", "cst", "ones", "xt", "sq", "h", "ssq", "sd", "rstd", "posi", "ang", "us", "uc",
                                    "sinS", "cosT", "t1", "t2"]}
            Bps = [Buf() for _ in range(8)]
            Bqk = [Buf(), Buf()]
            Bgt = [Buf() for _ in range(4)]
            Bvt = [Buf(), Buf()]
            R.op("dve", lambda e: e.memset(ones_f[:], 1.0), writes=[B["ones"]])
            for kc in range(KC):
                R.dma("pool", W[:, kc, :], wB[kc * 128:(kc + 1) * 128, :], writes=[B["W"]], chan="w")
            R.dma("sp", g1s[:], g1, writes=[B["g1s"]], chan="c0")
            R.dma("sp", cst[:], cst_d, writes=[B["cst"]], chan="c0")
            xv = xT.rearrange("(k p) t -> p k t", p=128)
            for ti in range(TN):
                tsl = slice(ti * TT, (ti + 1) * TT)
                R.dma("sp", xt[:].rearrange("p (k t) -> p k t", k=KC), xv[:, :, tsl], writes=[B["xt"]], chan="x")
                R.dma("sp", posi[:], pos[0:1, tsl].partition_broadcast(128), writes=[B["posi"]], chan="p")
                _rmsnorm_rstd(R, xt[:], sq, ssq, ps[0][:], rstd, sd, ones_f, cst, {**B, "ps_r": Bps[0]})
                for kc in range(KC):
                    R.op("dve", lambda e, kc=kc: e.scalar_tensor_tensor(
                        out=h[:, kc, :], in0=xt[:, kc * TT:(kc + 1) * TT], scalar=g1s[:, kc:kc + 1], in1=rstd[:],
                        op0=ALU.mult, op1=ALU.mult), reads=[B["xt"], B["g1s"], B["rstd"]], writes=[B["h"]])
                R.op("dve", lambda e: e.tensor_copy(out=ang[:], in_=posi[:]), reads=[B["posi"]], writes=[B["ang"]])
                R.op("dve", lambda e: e.tensor_scalar(out=ang[:], in0=ang[:], scalar1=cst[:, 1:2], scalar2=None, op0=ALU.mult),
                     reads=[B["ang"], B["cst"]], writes=[B["ang"]])
                R.op("dve", lambda e: e.tensor_scalar(out=t1[:], in0=ang[:], scalar1=1.0 / (2 * PI), scalar2=None, op0=ALU.mult),
                     reads=[B["ang"]], writes=[B["t1"]])
                R.op("dve", lambda e: e.tensor_copy(out=posi[:], in_=t1[:]), reads=[B["t1"]], writes=[B["posi"]])
                R.op("dve", lambda e: e.tensor_copy(out=t1[:], in_=posi[:]), reads=[B["posi"]], writes=[B["t1"]])
                R.op("dve", lambda e: e.scalar_tensor_tensor(out=us[:], in0=t1[:], scalar=-2 * PI, in1=ang[:], op0=ALU.mult, op1=ALU.add),
                     reads=[B["t1"], B["ang"]], writes=[B["us"]])
                R.op("dve", lambda e: e.tensor_scalar(out=t2[:], in0=us[:], scalar1=PI, scalar2=None, op0=ALU.is_gt),
                     reads=[B["us"]], writes=[B["t2"]])
                R.op("dve", lambda e: e.scalar_tensor_tensor(out=us[:], in0=t2[:], scalar=-2 * PI, in1=us[:], op0=ALU.mult, op1=ALU.add),
                     reads=[B["t2"], B["us"]], writes=[B["us"]])
                R.op("dve", lambda e: e.tensor_scalar(out=uc[:], in0=us[:], scalar1=0.5 * PI, scalar2=None, op0=ALU.add),
                     reads=[B["us"]], writes=[B["uc"]])
                R.op("dve", lambda e: e.tensor_scalar(out=t2[:], in0=uc[:], scalar1=PI, scalar2=None, op0=ALU.is_gt),
                     reads=[B["uc"]], writes=[B["t2"]])
                R.op("dve", lambda e: e.scalar_tensor_tensor(out=uc[:], in0=t2[:], scalar=-2 * PI, in1=uc[:], op0=ALU.mult, op1=ALU.add),
                     reads=[B["t2"], B["uc"]], writes=[B["uc"]])
                R.op("act", lambda e: e.activation(out=sinS[:], in_=us[:], func=AF.Sin, scale=cst[:, 2:3]),
                     reads=[B["us"], B["cst"]], writes=[B["sinS"]])
                R.op("act", lambda e: e.activation(out=cosT[:], in_=uc[:], func=AF.Sin),
                     reads=[B["uc"], B["cst"]], writes=[B["cosT"]])

                def fm(gi, pb):
                    for kc in range(KC):
                        R.op("pe", lambda e, kc=kc: e.matmul(ps[pb][:], lhsT=W[:, kc, gi * 128:(gi + 1) * 128], rhs=h[:, kc, :],
                                                             start=(kc == 0), stop=(kc == KC - 1)),
                             reads=[B["W"], B["h"]], writes=[Bps[pb]], nosig=(kc < KC - 1))
                for j, dst, dn in ((0, QTd, "QTd"), (1, KTd, "KTd")):
                    fm(2 * j, 1)
                    fm(2 * j + 1, 2)
                    R.op("dve", lambda e: e.tensor_tensor(out=t1[:], in0=ps[1][:], in1=cosT[:], op=ALU.mult),
                         reads=[Bps[1], B["cosT"]], writes=[B["t1"]])
                    R.op("dve", lambda e: e.tensor_tensor(out=t2[:], in0=ps[2][:], in1=sinS[:], op=ALU.mult),
                         reads=[Bps[2], B["sinS"]], writes=[B["t2"]])
                    R.op("pool", lambda e, j=j: e.tensor_tensor(out=qk[j][:], in0=t1[:], in1=t2[:], op=ALU.add),
                         reads=[B["t1"], B["t2"]], writes=[Bqk[j]])
                    R.dma("sp", dst[:, tsl], qk[j][:], reads=[Bqk[j]], writes=[BD[dn]], chan="o" + dn)
                for j, (gi, fn_, dst, dn) in enumerate(((4, AF.Silu, QHd, "QHd"), (5, AF.Sigmoid, SFd, "SFd"),
                                                        (6, AF.Sigmoid, SBd, "SBd"), (7, AF.Silu, SGd, "SGd"))):
                    pb = 3 + j
                    fm(gi, pb)
                    R.op("act", lambda e, j=j, pb=pb, fn_=fn_: e.activation(out=gt[j][:], in_=ps[pb][:], func=fn_),
                         reads=[Bps[pb]], writes=[Bgt[j]])
                    R.dma("sp", dst[:, tsl], gt[j][:], reads=[Bgt[j]], writes=[BD[dn]], chan="o" + dn)
                for ts in range(4):
                    pb = 7 if ts % 2 == 0 else 0
                    vb = ts % 2
                    for kc in range(KC):
                        R.op("pe", lambda e, kc=kc, ts=ts, pb=pb: e.matmul(
                            ps[pb][:, 0:256], lhsT=h[:, kc, ts * 128:(ts + 1) * 128], rhs=W[:, kc, 1024:1280],
                            start=(kc == 0), stop=(kc == KC - 1)), reads=[B["W"], B["h"]], writes=[Bps[pb]],
                            nosig=(kc < KC - 1))
                    R.op("act", lambda e, pb=pb, vb=vb: e.activation(out=vt[vb][:], in_=ps[pb][:, 0:256], func=AF.Copy),
                         reads=[Bps[pb]], writes=[Bvt[vb]])
                    r0 = ti * TT + ts * 128
                    R.dma("sp", Vd[r0:r0 + 128, :], vt[vb][:, 0:128], reads=[Bvt[vb]], writes=[BD["Vd"]], chan="oV%d" % vb)
                    R.dma("sp", HId[r0:r0 + 128, :], vt[vb][:, 128:256], reads=[Bvt[vb]], writes=[BD["HId"]], chan="oV%d" % vb)
            R.flush()
        with ExitStack() as es:
            sb = lambda n, s, d: es.enter_context(nc.sbuf_tensor("b_" + n, s, d))
            QT = sb("QT", [128, T], BF16)
            KT = sb("KT", [128, T], BF16)
            V = sb("V", [128, NKC, 128], BF16)
            cst = sb("cst", [128, 16], F32)
            dl = sb("dl", [128, 256], F32)
            sc = sb("sc", [128, 16], F32)
            tmp = sb("tmp", [128, 128], F32)
            ones_f = sb("ones_f", [128, 128], F32)
            ones_b = sb("ones_b", [128, 128], BF16)
            PT = [[sb("PT%d%d" % (m, i), [128, TT], BF16) for i in range(2)] for m in range(2)]
            rz = [sb("rz%d" % i, [128, TT], F32) for i in range(2)]
            o12 = [sb("o12%d" % i, [128, TT], F32) for i in range(2)]
            oo = sb("oo", [128, TT], F32)
            sq = sb("sq", [128, TT], F32)
            sd = sb("sd", [128, TT], F32)
            rstd = sb("rstd", [128, TT], F32)
            ot = sb("ot", [128, TT], F32)
            ps = [es.enter_context(nc.psum_tensor("bps%d" % i, [128, TT], F32)) for i in range(8)]
            B = {k: Buf() for k in ["QT", "KT", "V", "cst", "dl", "sc", "tmp", "ones", "oo", "sq", "sd", "rstd", "ot"]}
            Bps = [Buf() for _ in range(8)]
            BPT = [[Buf(), Buf()], [Buf(), Buf()]]
            Brz = [Buf(), Buf()]
            Bo12 = [Buf(), Buf()]
            R.op("dve", lambda e: e.memset(ones_f[:], 1.0), writes=[B["ones"]])
            R.op("dve", lambda e: e.memset(ones_b[:], 1.0), writes=[B["ones"]])
            R.dma("sp", cst[:], cst_d, writes=[B["cst"]], chan="c0")
            R.dma("sp", dl[:], dl_d[0:1, :].partition_broadcast(128), writes=[B["dl"]], chan="c0")
            nsp = max(1, T // 2048)
            w_ = T // nsp
            for i in range(nsp):
                R.dma("sp", QT[:, i * w_:(i + 1) * w_], QTd[:, i * w_:(i + 1) * w_], reads=[BD["QTd"]], writes=[B["QT"]], chan="lq")
                R.dma("sp", KT[:, i * w_:(i + 1) * w_], KTd[:, i * w_:(i + 1) * w_], reads=[BD["KTd"]], writes=[B["KT"]], chan="lk")
            Vv = Vd.rearrange("(c p) e -> p c e", p=128)
            cw = NKC // nsp
            for i in range(nsp):
                R.dma("sp", V[:, i * cw:(i + 1) * cw, :], Vv[:, i * cw:(i + 1) * cw, :], reads=[BD["Vd"]], writes=[B["V"]], chan="lv")
            for j in range(2):
                R.op("dve", lambda e, j=j: e.tensor_tensor(out=tmp[:, 0:64], in0=dl[:, 128 * j:128 * j + 64],
                                                           in1=dl[:, 128 * j + 64:128 * j + 128], op=ALU.mult),
                     reads=[B["dl"]], writes=[B["tmp"]])
                R.op("dve", lambda e, j=j: e.tensor_reduce(out=sc[:, j:j + 1], in_=tmp[:, 0:64], axis=AX.X, op=ALU.add),
                     reads=[B["tmp"]], writes=[B["sc"]])
            R.op("act", lambda e: e.activation(out=sc[:, 2:4], in_=sc[:, 0:2], func=AF.Exp), reads=[B["sc"]], writes=[B["sc"]])
            R.op("dve", lambda e: e.tensor_tensor(out=sc[:, 4:5], in0=sc[:, 3:4], in1=sc[:, 2:3], op=ALU.subtract),
                 reads=[B["sc"]], writes=[B["sc"]])
            R.op("dve", lambda e: e.tensor_tensor(out=sc[:, 4:5], in0=sc[:, 4:5], in1=cst[:, 5:6], op=ALU.subtract),
                 reads=[B["sc"], B["cst"]], writes=[B["sc"]])
            R.op("dve", lambda e: e.tensor_tensor(out=sc[:, 5:6], in0=cst[:, 7:8], in1=cst[:, 6:7], op=ALU.mult),
                 reads=[B["cst"], B["sc"]], writes=[B["sc"]])
            for qi in range(TN):
                qsl = slice(qi * TT, (qi + 1) * TT)
                for kc in range(NKC):
                    ksl = slice(kc * 128, (kc + 1) * 128)
                    bfi = kc % 2
                    for m in range(2):
                        pb = 2 * m + bfi
                        R.op("pe", lambda e, m=m, pb=pb, ksl=ksl, qsl=qsl: e.matmul(
                            ps[pb][:], lhsT=KT[m * 64:(m + 1) * 64, ksl], rhs=QT[m * 64:(m + 1) * 64, qsl], start=True, stop=True),
                            reads=[B["KT"], B["QT"]], writes=[Bps[pb]])
                    for m in range(2):
                        pb = 2 * m + bfi
                        R.op("act", lambda e, m=m, pb=pb, bfi=bfi: e.activation(out=PT[m][bfi][:], in_=ps[pb][:], func=AF.Exp, scale=0.125),
                             reads=[Bps[pb]], writes=[BPT[m][bfi]])
                    for m in range(2):
                        R.op("pe", lambda e, m=m, kc=kc, bfi=bfi: e.matmul(
                            ps[4 + m][:], lhsT=V[:, kc, :], rhs=PT[m][bfi][:], start=(kc == 0), stop=(kc == NKC - 1)),
                            reads=[B["V"], BPT[m][bfi]], writes=[Bps[4 + m]], nosig=True)
                        R.op("pe", lambda e, m=m, kc=kc, bfi=bfi: e.matmul(
                            ps[6 + m][:], lhsT=ones_b[:], rhs=PT[m][bfi][:], start=(kc == 0), stop=(kc == NKC - 1)),
                            reads=[B["ones"], BPT[m][bfi]], writes=[Bps[6 + m]])
                for m in range(2):
                    R.op("dve", lambda e, m=m: e.reciprocal(out=rz[m][:], in_=ps[6 + m][:]), reads=[Bps[6 + m]], writes=[Brz[m]])
                    R.op("dve", lambda e, m=m: e.tensor_tensor(out=o12[m][:], in0=ps[4 + m][:], in1=rz[m][:], op=ALU.mult),
                         reads=[Bps[4 + m], Brz[m]], writes=[Bo12[m]])
                R.op("dve", lambda e: e.scalar_tensor_tensor(out=oo[:], in0=o12[1][:], scalar=sc[:, 4:5], in1=o12[0][:],
                                                             op0=ALU.mult, op1=ALU.add),
                     reads=[Bo12[0], Bo12[1], B["sc"]], writes=[B["oo"]])
                R.op("act", lambda e: e.activation(out=sq[:], in_=oo[:], func=AF.Square), reads=[B["oo"]], writes=[B["sq"]])
                R.op("pe", lambda e: e.matmul(ps[0][:], lhsT=ones_f[:], rhs=sq[:], start=True, stop=True),
                     reads=[B["ones"], B["sq"]], writes=[Bps[0]])
                R.op("act", lambda e: e.activation(out=sd[:], in_=ps[0][:], func=AF.Sqrt, scale=1.0 / 128, bias=cst[:, 0:1]),
                     reads=[Bps[0], B["cst"]], writes=[B["sd"]])
                R.op("dve", lambda e: e.reciprocal(out=rstd[:], in_=sd[:]), reads=[B["sd"]], writes=[B["rstd"]])
                R.op("dve", lambda e: e.scalar_tensor_tensor(out=ot[:], in0=oo[:], scalar=sc[:, 5:6], in1=rstd[:],
                                                             op0=ALU.mult, op1=ALU.mult),
                     reads=[B["oo"], B["sc"], B["rstd"]], writes=[B["ot"]])
                R.dma("sp", mixT[0:128, qsl], ot[:], reads=[B["ot"]], chan="mo")
            R.flush()
        with ExitStack() as es:
            sb = lambda n, s, d: es.enter_context(nc.sbuf_tensor("c_" + n, s, d))
            cst = sb("cst", [128, 16], F32)
            lbl = sb("lbl", [128, 8], F32)
            el = sb("el", [128, 8], F32)
            lb = sb("lb", [128, 16], F32)
            tri = sb("tri", [64, 128], F32)
            cmask = sb("cmask", [128, TT], F32)
            ident = sb("ident", [128, 128], BF16)
            ones_f = sb("ones_f", [128, 128], F32)
            names = ["sg_", "qh", "f", "logf", "kk", "b", "nb", "gx", "eb", "enb", "otile", "of", "sgt", "osum", "sq", "sd", "rstd", "res", "res2"]
            tl = {n: sb(n, [128, TT], F32) for n in names}
            qb = sb("qb", [128, TT], BF16)
            kb = sb("kb", [128, TT], BF16)
            hi = sb("hi", [64, TT // 64, 128], BF16)
            kbT = sb("kbT", [64, 128], BF16)
            AT = sb("AT", [64, 64], BF16)
            S = sb("S", [128, 128], F32)
            S2 = sb("S2", [128, 128], F32)
            Sb = sb("Sb", [128, 128], BF16)
            psT = es.enter_context(nc.psum_tensor("cpsT", [128, 1024], BF16))
            psA = es.enter_context(nc.psum_tensor("cpsA", [128, TT], F32))
            pso = es.enter_context(nc.psum_tensor("cpso", [128, TT], F32))
            psS = es.enter_context(nc.psum_tensor("cpsS", [128, TT], F32))
            psn = es.enter_context(nc.psum_tensor("cpsn", [128, TT], F32))
            B = {k: Buf() for k in names + ["cst", "lbl", "el", "lb", "tri", "cmask", "ident", "ones", "qb", "kb", "hi", "kbT", "AT",
                                            "S", "S2", "Sb", "psT", "psA", "pso", "psS", "psn"]}
            R.op("dve", lambda e: e.memset(ones_f[:], 1.0), writes=[B["ones"]])
            R.dma("sp", cst[:], cst_d, writes=[B["cst"]], chan="c0")
            R.dma("sp", lbl[:], lbl_d, writes=[B["lbl"]], chan="c0")
            R.dma("sp", tri[:], tri_d, writes=[B["tri"]], chan="c0")
            R.dma("sp", cmask[:], cmask_d, writes=[B["cmask"]], chan="c0")
            R.dma("pool", ident[:], ident_d, writes=[B["ident"]], chan="c1")
            R.op("act", lambda e: e.activation(out=el[:], in_=lbl[:], func=AF.Exp), reads=[B["lbl"]], writes=[B["el"]])
            for d in range(2):
                c0 = 4 * d
                R.op("dve", lambda e, d=d, c0=c0: e.tensor_reduce(out=lb[:, c0 + 3:c0 + 4], in_=el[:, 4 * d:4 * d + 4], axis=AX.X, op=ALU.add),
                     reads=[B["el"]], writes=[B["lb"]])
                R.op("dve", lambda e, d=d: e.tensor_tensor(out=el[:, 4 * d:4 * d + 4], in0=el[:, 4 * d:4 * d + 4], in1=cst[:, 9:13], op=ALU.mult),
                     reads=[B["el"], B["cst"], B["lb"]], writes=[B["el"]])
                R.op("dve", lambda e, d=d, c0=c0: e.tensor_reduce(out=lb[:, c0:c0 + 1], in_=el[:, 4 * d:4 * d + 4], axis=AX.X, op=ALU.add),
                     reads=[B["el"]], writes=[B["lb"]])
                R.op("dve", lambda e, c0=c0: e.reciprocal(out=lb[:, c0 + 3:c0 + 4], in_=lb[:, c0 + 3:c0 + 4]), reads=[B["lb"]], writes=[B["lb"]])
                R.op("dve", lambda e, c0=c0: e.tensor_tensor(out=lb[:, c0:c0 + 1], in0=lb[:, c0:c0 + 1], in1=lb[:, c0 + 3:c0 + 4], op=ALU.mult),
                     reads=[B["lb"]], writes=[B["lb"]])
                R.op("dve", lambda e, c0=c0: e.tensor_scalar(out=lb[:, c0 + 1:c0 + 2], in0=lb[:, c0:c0 + 1], scalar1=-1.0, scalar2=1.0,
                                                             op0=ALU.mult, op1=ALU.add), reads=[B["lb"]], writes=[B["lb"]])
                R.op("dve", lambda e, c0=c0: e.tensor_scalar(out=lb[:, c0 + 2:c0 + 3], in0=lb[:, c0 + 1:c0 + 2], scalar1=-1.0, scalar2=None,
                                                             op0=ALU.mult), reads=[B["lb"]], writes=[B["lb"]])
            NCH = TT // 64
            HIv = HId.rearrange("(c s) e -> s c e", s=64)
            for d in range(2):
                c0 = 4 * d
                SGsrc, sname = (SFd, "SFd") if d == 0 else (SBd, "SBd")
                R.op("dve", lambda e: e.memset(S[:], 0.0), reads=[B["S"]], writes=[B["S"]])
                R.op("dve", lambda e: e.memset(Sb[:], 0.0), reads=[B["Sb"]], writes=[B["Sb"]])
                tiles = range(TN) if d == 0 else range(TN - 1, -1, -1)
                for ti in tiles:
                    tsl = slice(ti * TT, (ti + 1) * TT)
                    R.dma("sp", tl["sg_"][:], SGsrc[:, tsl], reads=[BD[sname]], writes=[B["sg_"]], chan="h0")
                    R.dma("sp", tl["qh"][:], QHd[:, tsl], reads=[BD["QHd"]], writes=[B["qh"]], chan="h1")
                    R.dma("sp", hi[:], HIv[:, ti * NCH:(ti + 1) * NCH, :], reads=[BD["HId"]], writes=[B["hi"]], chan="h2")
                    if d == 1:
                        R.dma("sp", tl["of"][:], OFd[:, tsl], reads=[BD["OFd"]], writes=[B["of"]], chan="h3")
                        R.dma("sp", tl["sgt"][:], SGd[:, tsl], reads=[BD["SGd"]], writes=[B["sgt"]], chan="h4")
                    R.op("dve", lambda e, c0=c0: e.tensor_scalar(out=tl["f"][:], in0=tl["sg_"][:], scalar1=lb[:, c0 + 1:c0 + 2],
                                                                 scalar2=lb[:, c0:c0 + 1], op0=ALU.mult, op1=ALU.add),
                         reads=[B["sg_"], B["lb"]], writes=[B["f"]])
                    R.op("act", lambda e: e.activation(out=tl["logf"][:], in_=tl["f"][:], func=AF.Ln), reads=[B["f"]], writes=[B["logf"]])
                    R.op("dve", lambda e, c0=c0: e.tensor_scalar(out=tl["kk"][:], in0=tl["sg_"][:], scalar1=lb[:, c0 + 2:c0 + 3],
                                                                 scalar2=lb[:, c0 + 1:c0 + 2], op0=ALU.mult, op1=ALU.add),
                         reads=[B["sg_"], B["lb"]], writes=[B["kk"]])
                    R.op("dve", lambda e: e.tensor_tensor_scan(out=tl["b"][:], data0=cmask[:], data1=tl["logf"][:], initial=0.0,
                                                               op0=ALU.mult, op1=ALU.add),
                         reads=[B["cmask"], B["logf"]], writes=[B["b"]])
                    if d == 0:
                        gx, gname = tl["b"], "b"
                    else:
                        R.op("dve", lambda e: e.tensor_tensor(out=tl["nb"][:], in0=tl["logf"][:], in1=tl["b"][:], op=ALU.subtract),
                             reads=[B["logf"], B["b"]], writes=[B["nb"]])
                        for j in range(NCH):
                            R.op("dve", lambda e, j=j: e.tensor_scalar(out=tl["gx"][:, j * 64:(j + 1) * 64], in0=tl["nb"][:, j * 64:(j + 1) * 64],
                                                                       scalar1=tl["b"][:, j * 64 + 63:j * 64 + 64], scalar2=None, op0=ALU.add),
                                 reads=[B["nb"], B["b"]], writes=[B["gx"]])
                        gx, gname = tl["gx"], "gx"
                    R.op("act", lambda e, gx=gx: e.activation(out=tl["eb"][:], in_=gx[:], func=AF.Exp), reads=[B[gname]], writes=[B["eb"]])
                    R.op("act", lambda e, gx=gx: e.activation(out=tl["enb"][:], in_=gx[:], func=AF.Exp, scale=-1.0),
                         reads=[B[gname]], writes=[B["enb"]])
                    R.op("dve", lambda e: e.scalar_tensor_tensor(out=qb[:], in0=tl["qh"][:], scalar=128.0 ** -0.5, in1=tl["eb"][:],
                                                                 op0=ALU.mult, op1=ALU.mult), reads=[B["qh"], B["eb"]], writes=[B["qb"]])
                    R.op("pool", lambda e: e.tensor_tensor(out=kb[:], in0=tl["kk"][:], in1=tl["enb"][:], op=ALU.mult),
                         reads=[B["kk"], B["enb"]], writes=[B["kb"]])
                    chunks = range(NCH) if d == 0 else range(NCH - 1, -1, -1)
                    for j in chunks:
                        csl = slice(j * 64, (j + 1) * 64)
                        dcol = j * 64 + 63 if d == 0 else j * 64
                        R.op("pe", lambda e, csl=csl: e.transpose(out=psT[0:64, 0:128], in_=kb[:, csl], identity=ident[:]),
                             reads=[B["kb"], B["ident"]], writes=[B["psT"]])
                        R.op("act", lambda e: e.activation(out=kbT[:], in_=psT[0:64, 0:128], func=AF.Copy), reads=[B["psT"]], writes=[B["kbT"]])
                        R.op("pe", lambda e, csl=csl: e.matmul(psA[0:64, 0:64], lhsT=kb[:, csl], rhs=qb[:, csl], start=True, stop=True),
                             reads=[B["kb"], B["qb"]], writes=[B["psA"]])
                        R.op("dve", lambda e, d=d: e.tensor_tensor(out=AT[:], in0=psA[0:64, 0:64], in1=tri[:, 64 * d:64 * d + 64], op=ALU.mult),
                             reads=[B["psA"], B["tri"]], writes=[B["AT"]])
                        R.op("pe", lambda e, j=j: e.matmul(pso[:, 0:64], lhsT=hi[:, j, :], rhs=AT[:], start=True, stop=False),
                             reads=[B["hi"], B["AT"]], writes=[B["pso"]], nosig=True)
                        R.op("pe", lambda e, csl=csl: e.matmul(pso[:, 0:64], lhsT=Sb[:], rhs=qb[:, csl], start=False, stop=True),
                             reads=[B["Sb"], B["qb"]], writes=[B["pso"]])
                        R.op("act", lambda e, csl=csl: e.activation(out=tl["otile"][:, csl], in_=pso[:, 0:64], func=AF.Copy),
                             reads=[B["pso"]], writes=[B["otile"]])
                        R.op("pe", lambda e, j=j: e.matmul(psS[:, 0:128], lhsT=kbT[:], rhs=hi[:, j, :], start=True, stop=True),
                             reads=[B["kbT"], B["hi"]], writes=[B["psS"]])
                        R.op("dve", lambda e: e.tensor_tensor(out=S2[:], in0=psS[:, 0:128], in1=S[:], op=ALU.add),
                             reads=[B["psS"], B["S"]], writes=[B["S2"]])
                        R.op("dve", lambda e, dcol=dcol: e.tensor_scalar(out=S[:], in0=S2[:], scalar1=tl["eb"][:, dcol:dcol + 1], scalar2=None,
                                                                         op0=ALU.mult), reads=[B["S2"], B["eb"]], writes=[B["S"]])
                        R.op("pool", lambda e: e.tensor_copy(out=Sb[:], in_=S[:]), reads=[B["S"]], writes=[B["Sb"]])
                    if d == 0:
                        R.dma("sp", OFd[:, tsl], tl["otile"][:], reads=[B["otile"]], writes=[BD["OFd"]], chan="h5")
                    else:
                        R.op("pool", lambda e: e.tensor_tensor(out=tl["osum"][:], in0=tl["otile"][:], in1=tl["of"][:], op=ALU.add),
                             reads=[B["otile"], B["of"]], writes=[B["osum"]])
                        R.op("act", lambda e: e.activation(out=tl["sq"][:], in_=tl["osum"][:], func=AF.Square), reads=[B["osum"]], writes=[B["sq"]])
                        R.op("pe", lambda e: e.matmul(psn[:], lhsT=ones_f[:], rhs=tl["sq"][:], start=True, stop=True),
                             reads=[B["ones"], B["sq"]], writes=[B["psn"]])
                        R.op("act", lambda e: e.activation(out=tl["sd"][:], in_=psn[:], func=AF.Sqrt, scale=1.0 / 128, bias=cst[:, 0:1]),
                             reads=[B["psn"], B["cst"]], writes=[B["sd"]])
                        R.op("dve", lambda e: e.reciprocal(out=tl["rstd"][:], in_=tl["sd"][:]), reads=[B["sd"]], writes=[B["rstd"]])
                        R.op("dve", lambda e: e.scalar_tensor_tensor(out=tl["res"][:], in0=tl["osum"][:], scalar=cst[:, 8:9], in1=tl["rstd"][:],
                                                                     op0=ALU.mult, op1=ALU.mult),
                             reads=[B["osum"], B["cst"], B["rstd"]], writes=[B["res"]])
                        R.op("pool", lambda e: e.tensor_tensor(out=tl["res2"][:], in0=tl["res"][:], in1=tl["sgt"][:], op=ALU.mult),
                             reads=[B["res"], B["sgt"]], writes=[B["res2"]])
                        R.dma("sp", mixT[128:256, tsl], tl["res2"][:], reads=[B["res2"]], chan="mo2")
            R.flush()
    return nc


def _r16(g):
    return np.ascontiguousarray(np.asarray(g, np.float32).reshape(KC, 128).T)


def _swap64(cols):
    c = np.asarray(cols).reshape(-1, 2, 32)
    return c[:, ::-1, :].reshape(-1)


def _b_consts(layer, subln_g, hnorm_g):
    inv = (10000.0 ** (-np.arange(0, 64, 2, dtype=np.float32) / 64)).astype(np.float32)
    p = np.arange(128)
    cst = np.zeros((128, 16), np.float32)
    cst[:, 0] = EPS
    cst[:, 1] = inv[p % 32]
    sgn = np.where((p % 64) < 32, -1.0, 1.0).astype(np.float32)
    cst[:, 2] = sgn
    cst[:, 3] = -PI * sgn
    cst[:, 4] = -PI
    lam_init = 0.8 - 0.6 * math.exp(-0.3 * layer)
    cst[:, 5] = lam_init
    cst[:, 6] = 1.0 - lam_init
    cst[:, 7] = subln_g
    cst[:, 8] = hnorm_g
    for j in range(4):
        cst[:, 9 + j] = 1.0 if 1 <= j <= layer else 0.0
    return cst


def _b_inputs(c, layer, xT, positions, norm1_g, w_in_l, diff_lambda_l, subln_g_l, lb_logits, hnorm_g_l):
    q = np.arange(c * 128, (c + 1) * 128)
    cols = np.concatenate([q, _swap64(q), 1024 + q, _swap64(1024 + q), 3072 + q, 4096 + q, 5120 + q, 7168 + q,
                           2048 + q, 6144 + q])
    tri = np.zeros((64, 128), np.float32)
    s = np.arange(64)[:, None]
    t = np.arange(64)[None, :]
    tri[:, 0:64] = (s <= t)
    tri[:, 64:128] = (s >= t)
    cmask = np.ones((128, TT), np.float32)
    cmask[:, ::64] = 0.0
    lbl = np.ascontiguousarray(lb_logits[:, :, c * 128:(c + 1) * 128].transpose(2, 0, 1).reshape(128, 8)).astype(np.float32)
    return {"xT": xT, "wB": np.ascontiguousarray(w_in_l[:, cols]), "g1": _r16(norm1_g),
            "pos": np.ascontiguousarray(np.asarray(positions, np.int32).reshape(1, -1)),
            "dl": np.ascontiguousarray(np.asarray(diff_lambda_l, np.float32).reshape(1, 256)),
            "cst": _b_consts(layer, subln_g_l, hnorm_g_l), "lbl": lbl, "tri": tri, "cmask": cmask,
            "ident": np.eye(128, dtype=np.float32)}


_NC_CACHE = {}


def _get(kind, *a):
    k = (kind,) + a
    if k not in _NC_CACHE:
        _NC_CACHE[k] = {"B": build_B, "C": build_C, "D": build_D}[kind](*a)
    return _NC_CACHE[k]


def _forward(x, positions, norm1_g, w_in, diff_lambda, diff_subln_g, hgrn_lb_logits, hgrn_norm_g, w_out, norm2_g,
             router_w, expert_w_gate, expert_w_up, expert_w_down, final_norm_g, depth=4):
    T = x.shape[1]
    TL = T // NCORES
    SEG = T // 8
    cores = list(range(NCORES))
    xT = np.ascontiguousarray(np.asarray(x, np.float32)[0].T)
    cstc = np.zeros((128, 8), np.float32)
    cstc[:, 0] = EPS
    gm = np.kron(np.eye(16), np.ones((8, 8))).astype(np.float32)
    sel = np.zeros((128, 16), np.float32)
    sel[np.arange(16) * 8, np.arange(16)] = 1.0
    yT = None
    for l in range(depth):
        ncB = _get("B", T)
        ims = [_b_inputs(c, l, xT, positions, norm1_g[l], np.asarray(w_in[l]), diff_lambda[l], diff_subln_g[l],
                         np.asarray(hgrn_lb_logits), hgrn_norm_g[l]) for c in cores]
        res = run_bass_kernel_spmd(ncB, ims, core_ids=cores).results
        mix = [r["mixT"] for r in res]
        mT = np.concatenate([m[0:128] for m in mix] + [m[128:256] for m in mix], axis=0)
        ncC = _get("C", TL)
        wo = np.asarray(w_out[l])
        rw = np.asarray(router_w[l])
        g2 = _r16(norm2_g[l])
        ims = [{"xT": np.ascontiguousarray(xT[:, c * TL:(c + 1) * TL]), "mT": np.ascontiguousarray(mT[:, c * TL:(c + 1) * TL]),
                "wo": wo, "g2": g2, "rw": rw, "cst": cstc} for c in cores]
        res = run_bass_kernel_spmd(ncC, ims, core_ids=cores).results
        x1T = np.concatenate([r["x1T"] for r in res], axis=1)
        aff = np.concatenate([r["aff"] for r in res], axis=0)
        afa = np.ascontiguousarray(aff.T.reshape(16, 8, SEG).reshape(128, SEG))
        ncD = _get("D", TL, T)
        wg_, wu_, wd_ = np.asarray(expert_w_gate[l]), np.asarray(expert_w_up[l]), np.asarray(expert_w_down[l])
        gf = _r16(final_norm_g)
        ims = [{"xT": np.ascontiguousarray(x1T[:, c * TL:(c + 1) * TL]), "g2": g2, "gf": gf, "afa": afa,
                "afl": np.ascontiguousarray(aff[c * TL:(c + 1) * TL].T), "wg": wg_, "wu": wu_, "wd": wd_,
                "cst": cstc, "gm": gm, "sel": sel} for c in cores]
        res = run_bass_kernel_spmd(ncD, ims, core_ids=cores).results
        xT = np.concatenate([r["x2T"] for r in res], axis=1)
        yT = [r["yT"] for r in res]
    y = np.concatenate(yT, axis=1).T
    return np.ascontiguousarray(y[None]).astype(np.float32)


def kernel(**inputs):
    inputs = {k: np.asarray(v) for k, v in inputs.items()}
    return _forward(**inputs)
```
